# Optimizing a Trainium2 kernel written in Bass

```python
import jax, jax.numpy as jnp
from jax import lax
import numpy as np

D_MODEL = 1024
BATCH = 4
SEQ = 4096
DEPTH = 1

GRID_W = 64
CTX_LEN = 256
RWKV_HEADS = 8
RWKV_HEAD_DIM = 64
RWKV_WIDTH = RWKV_HEADS * RWKV_HEAD_DIM
DECAY_LORA = 64
ICLR_LORA = 64
GATE_LORA = 128
SGU_WIDTH = 512
SGU_GROUPS = 8
CHUNK = 128
N_EXPERTS = 32
TOP_K = 4
D_EXPERT = D_MODEL
SWIGLU_LIMIT = 7.0
SWIGLU_ALPHA = 1.702
ROW_BLOCK = 128
N_BRANCHES = 2
RMS_EPS = 1e-6
LN_EPS = 1e-5
GN_EPS = 64e-5
RWKV_SPLIT = (RWKV_WIDTH, RWKV_WIDTH, RWKV_WIDTH, DECAY_LORA, DECAY_LORA, ICLR_LORA, ICLR_LORA, GATE_LORA)
RWKV_COLS = 3 * RWKV_WIDTH + 2 * DECAY_LORA + 2 * ICLR_LORA + GATE_LORA
SGU_COLS = 2 * SGU_WIDTH
GATE_COLS = N_BRANCHES * D_MODEL
IN_COLS = RWKV_COLS + SGU_COLS + GATE_COLS

kernel_name = 'hybrid_rwkv7_chunksgu_moe_dit_layer'


def _offsets(sizes):
    out, acc = [], 0
    for s in sizes[:-1]:
        acc += s
        out.append(acc)
    return out


def rmsnorm(x, g):
    xf = x.astype(jnp.float32)
    y = xf * lax.rsqrt(jnp.mean(xf * xf, axis=-1, keepdims=True) + RMS_EPS)
    return (y * g.astype(jnp.float32)).astype(x.dtype)


def layernorm(x, w, b, eps):
    xf = x.astype(jnp.float32)
    mu = jnp.mean(xf, axis=-1, keepdims=True)
    var = jnp.mean(jnp.square(xf - mu), axis=-1, keepdims=True)
    y = (xf - mu) * lax.rsqrt(var + eps)
    return (y * w.astype(jnp.float32) + b.astype(jnp.float32)).astype(x.dtype)


def modulate(h, shift, scale):
    return h * (1.0 + scale) + shift


def q_shift(p):
    B, T, C = p.shape
    rows = T // GRID_W
    g = p.reshape(B, rows, GRID_W, C // 4, 4)
    left = jnp.pad(g[:, :, :-1, :, 0], ((0, 0), (0, 0), (1, 0), (0, 0)))
    right = jnp.pad(g[:, :, 1:, :, 1], ((0, 0), (0, 0), (0, 1), (0, 0)))
    up = jnp.pad(g[:, :-1, :, :, 2], ((0, 0), (1, 0), (0, 0), (0, 0)))
    down = jnp.pad(g[:, 1:, :, :, 3], ((0, 0), (0, 1), (0, 0), (0, 0)))
    return jnp.stack([left, right, up, down], axis=-1).reshape(B, T, C)


def seq_shift(p):
    B, T, C = p.shape
    g = p.reshape(B, T, C // 2, 2)
    prev = jnp.pad(g[:, :-1, :, 0], ((0, 0), (1, 0), (0, 0)))
    nxt = jnp.pad(g[:, 1:, :, 1], ((0, 0), (0, 1), (0, 0)))
    return jnp.stack([prev, nxt], axis=-1).reshape(B, T, C)


def rwkv_inputs(P, shift_fn, lp):
    P = P.astype(jnp.float32)
    P = P + lp['shift_mu'] * (shift_fn(P) - P)
    r, k, v, wd_f, wd_b, ad_f, ad_b, gd = jnp.split(P, _offsets(RWKV_SPLIT), axis=-1)
    B, T, _ = P.shape
    heads = lambda t: t.reshape(B, T, RWKV_HEADS, RWKV_HEAD_DIM)
    kk = heads(k * lp['k_k'])
    kk = kk * lax.rsqrt(jnp.sum(kk * kk, axis=-1, keepdims=True) + 1e-12)
    g = jax.nn.sigmoid(gd) @ lp['gate_lora_b']
    dirs = []
    for d, (wd, ad) in enumerate(((wd_f, ad_f), (wd_b, ad_b))):
        w_log = -jax.nn.softplus(-(lp['decay_w0'][d] + jnp.tanh(wd) @ lp['decay_lora_b'][d])) - 0.5
        decay = jnp.exp(-jnp.exp(w_log))
        a = jax.nn.sigmoid(lp['iclr_a0'][d] + ad @ lp['iclr_lora_b'][d])
        k_d = k * (1.0 + (a - 1.0) * lp['k_a'])
        dirs.append({'w': heads(decay), 'k': heads(k_d), 'a': -kk, 'b': kk * heads(a)})
    return heads(r), heads(v), g, dirs


def wkv_scan(S0, r, w, k, v, a, b, reverse, emit):
    tm = lambda t: jnp.moveaxis(t, 1, 0)

    def step(S, inp):
        r_t, w_t, k_t, v_t, a_t, b_t = inp
        sa = jnp.einsum('bhvk,bhk->bhv', S, a_t)
        S = S * w_t[:, :, None, :] + sa[..., None] * b_t[:, :, None, :] + v_t[..., None] * k_t[:, :, None, :]
        return S, (jnp.einsum('bhvk,bhk->bhv', S, r_t) if emit else None)

    S, y = lax.scan(step, S0, (tm(r), tm(w), tm(k), tm(v), tm(a), tm(b)), reverse=reverse)
    return S, (jnp.moveaxis(y, 0, 1) if emit else None)


def rwkv_readout(y, inputs, lp):
    r, v, g, dirs = inputs
    B, T = y.shape[:2]
    mu = jnp.mean(y, axis=-1, keepdims=True)
    var = jnp.mean(jnp.square(y - mu), axis=-1, keepdims=True)
    yn = ((y - mu) * lax.rsqrt(var + GN_EPS)).reshape(B, T, RWKV_WIDTH) * lp['gn_w'] + lp['gn_b']
    bonus = sum(jnp.sum(r * dd['k'] * lp['r_k'], axis=-1, keepdims=True) for dd in dirs) * v
    return ((yn + bonus.reshape(B, T, RWKV_WIDTH)) * g) @ lp['w_out_rwkv']


def rwkv_branch(P_ctx, P_lat, lp, ctx_out):
    ctx_in = rwkv_inputs(P_ctx, seq_shift, lp)
    lat_in = rwkv_inputs(P_lat, q_shift, lp)
    B = P_lat.shape[0]
    S0 = jnp.zeros((B, RWKV_HEADS, RWKV_HEAD_DIM, RWKV_HEAD_DIM), jnp.float32)
    y_lat, y_ctx = 0.0, 0.0
    for d, rev in enumerate((False, True)):
        rc, vc, _, dc = ctx_in
        S_c, yc = wkv_scan(S0, rc, dc[d]['w'], dc[d]['k'], vc, dc[d]['a'], dc[d]['b'], rev, ctx_out)
        rl, vl, _, dl = lat_in
        _, yl = wkv_scan(S_c, rl, dl[d]['w'], dl[d]['k'], vl, dl[d]['a'], dl[d]['b'], rev, True)
        y_lat = y_lat + yl
        if ctx_out:
            y_ctx = y_ctx + yc
    out_lat = rwkv_readout(y_lat, lat_in, lp).astype(P_lat.dtype)
    out_ctx = rwkv_readout(y_ctx, ctx_in, lp).astype(P_ctx.dtype) if ctx_out else None
    return out_lat, out_ctx


def chunk_sgu(P_sgu, lp):
    u, z = jnp.split(jax.nn.gelu(P_sgu, approximate=False), 2, axis=-1)
    z = layernorm(z, lp['sgu_ln_w'], lp['sgu_ln_b'], LN_EPS)
    B, T, C = z.shape
    zc = z.reshape(B, T // CHUNK, CHUNK, SGU_GROUPS, C // SGU_GROUPS)
    s = jnp.einsum('gpq,bnqgc->bnpgc', lp['sgu_w_spatial'], zc) + lp['sgu_b_spatial'].T[:, :, None]
    return u * s.reshape(B, T, C)


def merge_out(P_rest, y_a, lp):
    P_sgu, P_gate = jnp.split(P_rest, [SGU_COLS], axis=-1)
    y_b = chunk_sgu(P_sgu, lp) @ lp['w_out_sgu']
    gate_a, gate_b = jnp.split(jax.nn.sigmoid(P_gate), N_BRANCHES, axis=-1)
    return (gate_a * y_a + gate_b * y_b) @ lp['w_o']


def clamped_swiglu(gate, up):
    gate = jnp.minimum(gate, SWIGLU_LIMIT)
    up = jnp.clip(up, -SWIGLU_LIMIT, SWIGLU_LIMIT)
    return gate * jax.nn.sigmoid(SWIGLU_ALPHA * gate) * (up + 1.0)


def moe_ffn(h, lp):
    B, T, D = h.shape
    xf = h.reshape(-1, D)
    n_tok = xf.shape[0]
    logits = (xf @ lp['router_w'] + lp['router_b']).astype(jnp.float32)
    top_v, top_e = lax.top_k(logits, TOP_K)
    weights = jax.nn.softmax(top_v, axis=-1)
    n_assign = n_tok * TOP_K
    flat_e = top_e.reshape(-1)
    order = jnp.argsort(flat_e)
    sorted_e = flat_e[order]
    sorted_tok = (order // TOP_K).astype(jnp.int32)
    sorted_w = weights.reshape(-1)[order]
    counts = jnp.bincount(flat_e, length=N_EXPERTS)
    padded = (counts + ROW_BLOCK - 1) // ROW_BLOCK * ROW_BLOCK
    pad_end = jnp.cumsum(padded)
    pad_start = pad_end - padded
    grp_start = jnp.cumsum(counts) - counts
    dest = pad_start[sorted_e] + (jnp.arange(n_assign, dtype=jnp.int32) - grp_start[sorted_e])
    n_rows = (n_assign + N_EXPERTS * (ROW_BLOCK - 1) + ROW_BLOCK - 1) // ROW_BLOCK * ROW_BLOCK
    n_blocks = n_rows // ROW_BLOCK
    row_tok = jnp.zeros((n_rows,), jnp.int32).at[dest].set(sorted_tok)
    block_start = jnp.arange(n_blocks, dtype=jnp.int32) * ROW_BLOCK
    block_e = jnp.minimum(jnp.searchsorted(pad_end, block_start, side='right'), N_EXPERTS - 1)
    xb = xf[row_tok].reshape(n_blocks, ROW_BLOCK, D)

    def expert_block(args):
        xblk, e = args
        gate = xblk @ lp['exp_w_gate'][e] + lp['exp_b_gate'][e]
        up = xblk @ lp['exp_w_up'][e] + lp['exp_b_up'][e]
        return clamped_swiglu(gate, up) @ lp['exp_w_down'][e] + lp['exp_b_down'][e]

    yb = lax.map(expert_block, (xb, block_e)).reshape(n_rows, D)
    contrib = yb[dest] * sorted_w[:, None].astype(yb.dtype)
    out = jax.ops.segment_sum(contrib, sorted_tok, num_segments=n_tok)
    return out.reshape(B, T, D).astype(h.dtype)


def setup_inputs(seed: int = 0) -> dict:
    key = jax.random.key(seed)
    ks = iter(jax.random.split(key, 48))
    nrm = lambda shape, scale: jax.random.normal(next(ks), shape, jnp.float32) * scale
    L, D, W, E, F = DEPTH, D_MODEL, RWKV_WIDTH, N_EXPERTS, D_EXPERT
    return {
        'x': nrm((BATCH, SEQ, D), 1.0),
        'c': nrm((BATCH, D), 1.0),
        'ctx': nrm((BATCH, CTX_LEN, D), 1.0),
        'c_ctx': nrm((D,), 1.0),
        'w_ada': nrm((L, D, 6 * D), 0.5 * D ** -0.5),
        'b_ada': nrm((L, 6 * D), 0.02),
        'norm1_g': 1.0 + nrm((L, D), 0.01),
        'w_in': nrm((L, D, IN_COLS), D ** -0.5),
        'shift_mu': jax.random.uniform(next(ks), (L, RWKV_COLS), jnp.float32),
        'decay_w0': -2.0 + nrm((L, 2, W), 1.0),
        'decay_lora_b': nrm((L, 2, DECAY_LORA, W), 0.5 * DECAY_LORA ** -0.5),
        'iclr_a0': nrm((L, 2, W), 0.5),
        'iclr_lora_b': nrm((L, 2, ICLR_LORA, W), 0.5 * ICLR_LORA ** -0.5),
        'gate_lora_b': nrm((L, GATE_LORA, W), GATE_LORA ** -0.5),
        'k_k': 1.0 + nrm((L, W), 0.1),
        'k_a': 1.0 + nrm((L, W), 0.1),
        'r_k': nrm((L, RWKV_HEADS, RWKV_HEAD_DIM), 0.1),
        'gn_w': 1.0 + nrm((L, W), 0.01),
        'gn_b': nrm((L, W), 0.01),
        'w_out_rwkv': nrm((L, W, D), W ** -0.5),
        'sgu_ln_w': 1.0 + nrm((L, SGU_WIDTH), 0.01),
        'sgu_ln_b': nrm((L, SGU_WIDTH), 0.01),
        'sgu_w_spatial': nrm((L, SGU_GROUPS, CHUNK, CHUNK), CHUNK ** -0.5),
        'sgu_b_spatial': 1.0 + nrm((L, SGU_GROUPS, CHUNK), 0.1),
        'w_out_sgu': nrm((L, SGU_WIDTH, D), SGU_WIDTH ** -0.5),
        'w_o': nrm((L, D, D), D ** -0.5),
        'norm2_g': 1.0 + nrm((L, D), 0.01),
        'router_w': nrm((L, D, E), D ** -0.5),
        'router_b': nrm((L, E), 0.01),
        'exp_w_gate': nrm((L, E, D, F), D ** -0.5),
        'exp_b_gate': nrm((L, E, F), 0.01),
        'exp_w_up': nrm((L, E, D, F), D ** -0.5),
        'exp_b_up': nrm((L, E, F), 0.01),
        'exp_w_down': nrm((L, E, F, D), F ** -0.5),
        'exp_b_down': nrm((L, E, D), 0.01),
        'final_norm_g': 1.0 + nrm((D,), 0.01),
    }


def reference(x, c, ctx, c_ctx, w_ada, b_ada, norm1_g, w_in, shift_mu, decay_w0, decay_lora_b,
              iclr_a0, iclr_lora_b, gate_lora_b, k_k, k_a, r_k, gn_w, gn_b, w_out_rwkv,
              sgu_ln_w, sgu_ln_b, sgu_w_spatial, sgu_b_spatial, w_out_sgu, w_o, norm2_g,
              router_w, router_b, exp_w_gate, exp_b_gate, exp_w_up, exp_b_up, exp_w_down,
              exp_b_down, final_norm_g):
    for l in range(DEPTH):
        last = l == DEPTH - 1
        lp = {
            'shift_mu': shift_mu[l], 'decay_w0': decay_w0[l], 'decay_lora_b': decay_lora_b[l],
            'iclr_a0': iclr_a0[l], 'iclr_lora_b': iclr_lora_b[l], 'gate_lora_b': gate_lora_b[l],
            'k_k': k_k[l], 'k_a': k_a[l], 'r_k': r_k[l], 'gn_w': gn_w[l], 'gn_b': gn_b[l],
            'w_out_rwkv': w_out_rwkv[l], 'sgu_ln_w': sgu_ln_w[l], 'sgu_ln_b': sgu_ln_b[l],
            'sgu_w_spatial': sgu_w_spatial[l], 'sgu_b_spatial': sgu_b_spatial[l],
            'w_out_sgu': w_out_sgu[l], 'w_o': w_o[l], 'router_w': router_w[l],
            'router_b': router_b[l], 'exp_w_gate': exp_w_gate[l], 'exp_b_gate': exp_b_gate[l],
            'exp_w_up': exp_w_up[l], 'exp_b_up': exp_b_up[l], 'exp_w_down': exp_w_down[l],
            'exp_b_down': exp_b_down[l],
        }
        mod = jax.nn.silu(c) @ w_ada[l] + b_ada[l]
        mod_c = jax.nn.silu(c_ctx) @ w_ada[l] + b_ada[l]
        sh1, sc1, ga1, sh2, sc2, ga2 = [m[:, None, :] for m in jnp.split(mod, 6, axis=-1)]
        csh1, csc1, cga1, csh2, csc2, cga2 = jnp.split(mod_c, 6, axis=-1)

        h = modulate(rmsnorm(x, norm1_g[l]), sh1, sc1)
        hc = modulate(rmsnorm(ctx, norm1_g[l]), csh1, csc1)
        P = h @ w_in[l]
        Pc = hc @ (w_in[l, :, :RWKV_COLS] if last else w_in[l])
        y_a, y_a_ctx = rwkv_branch(Pc[..., :RWKV_COLS], P[..., :RWKV_COLS], lp, not last)
        x_new = x + ga1 * merge_out(P[..., RWKV_COLS:], y_a, lp)
        if not last:
            ctx = ctx + cga1 * merge_out(Pc[..., RWKV_COLS:], y_a_ctx, lp)
        x = x_new

        h = modulate(rmsnorm(x, norm2_g[l]), sh2, sc2)
        x = x + ga2 * moe_ffn(h, lp)
        if not last:
            hc = modulate(rmsnorm(ctx, norm2_g[l]), csh2, csc2)
            ctx = ctx + cga2 * moe_ffn(hc, lp)
    return rmsnorm(x, final_norm_g)
```

```python
import numpy as np
from contextlib import ExitStack
import concourse.bass as bass
import concourse.mybir as mybir
from concourse.bass_utils import run_bass_kernel_spmd

F32 = mybir.dt.float32
BF16 = mybir.dt.bfloat16
AF = mybir.ActivationFunctionType
OP = mybir.AluOpType
AX = mybir.AxisListType

import os
STOP = os.environ.get('KSTOP', '')
SUB = int(os.environ.get('KSUB', '0'))
MOE_DMA_ONLY = os.environ.get('KMOE', '') == 'dma'


class _Stop(Exception):
    pass


def ck(n):
    if STOP == 'B1' and SUB == n:
        raise _Stop()

SEM_LIMIT = 12000
SDT = mybir.dt.bfloat16
NCH, NOWN, NCTXC, NEXP = 64, 32, 4, 32
C0 = 0.6065306597126334

CP_G1, CP_G2, CP_B48, CP_MU, CP_ML, CP_MR, CP_MUP, CP_MDN, CP_CP, CP_CN = 0, 8, 16, 64, 79, 94, 109, 124, 139, 154
CP_KK, CP_KA, CP_W0, CP_A0, CP_RK = 169, 173, 177, 185, 193
NCP = 200
RC_LNW, RC_LNB, RC_RB, RC_FNG, RC_BGA, RC_GNW, RC_GNB = 0, 512, 1024, 1056, 2080, 4128, 4640
NRC = 5152


class Eng:
    def __init__(self, kb, name, eng):
        self.name, self.eng = name, eng
        self.sem = kb.new_sem('s_' + name)
        self.cnt = 0
        self.seen = {}
        self.n_ins = 0


class T:
    def __init__(self, kb, name, shape, dt, space='sb'):
        self.name = name
        if space == 'sb':
            self.t = kb.ctx.enter_context(kb.nc.sbuf_tensor('t_' + name, list(shape), dt))
        else:
            self.t = kb.ctx.enter_context(kb.nc.psum_tensor('t_' + name, list(shape), dt))
        self.w, self.r = [], []
        self.dsem, self.dcnt = None, 0
        self.is_psum = (space != 'sb')

    def __getitem__(self, idx):
        return self.t[idx]


class Sub(T):
    def __init__(self, parent, ap, name):
        self.name = name
        self.t = ap
        self.w, self.r = [], []
        self.dsem, self.dcnt = None, 0


class KB:
    def __init__(self, nc, ctx):
        self.nc, self.ctx = nc, ctx
        self.nsem = 0
        self.E = {}
        for name, eng in (('pe', nc.tensor), ('act', nc.scalar), ('dve', nc.vector),
                          ('pool', nc.gpsimd), ('sp', nc.sync)):
            self.E[name] = Eng(self, name, eng)
        self.dma_toks = {}
        self.banks = []
        self.bi = 0

    def new_sem(self, name):
        self.nsem += 1
        return self.ctx.enter_context(self.nc.semaphore(f'{name}_{self.nsem}'))

    def tile(self, name, shape, dt=F32):
        return T(self, name, shape, dt, 'sb')

    def nb(self):
        b = self.banks[self.bi % len(self.banks)]
        self.bi += 1
        return b

    def _wait(self, E, sem, val):
        k = id(sem)
        if E.seen.get(k, 0) >= val:
            return
        E.eng.wait_ge(sem, val)
        E.seen[k] = val

    def _dep1(self, E, tok, raw):
        sem, val, owner = tok
        if owner is E and E.name == 'pe':
            return
        self._wait(E, sem, val)

    def _deps(self, E, reads, writes, skip_own_dma=False):
        for t in reads:
            for tok in t.w:
                self._dep1(E, tok, True)
            if getattr(t, 'is_psum', False):
                for tok in t.r:
                    if tok[2] is not E:
                        self._wait(E, tok[0], tok[1])
        for t in writes:
            for tok in t.w:
                if skip_own_dma and t.dsem is not None and tok[0] is t.dsem:
                    continue
                self._dep1(E, tok, False)
            for tok in t.r:
                self._dep1(E, tok, False)

    def _token(self, E):
        if E.cnt >= SEM_LIMIT:
            E.sem = self.new_sem('s_' + E.name)
            E.cnt = 0
        E.cnt += 1
        return (E.sem, E.cnt, E)

    def _mark(self, tok, reads, writes):
        for t in reads:
            t.r.append(tok)
        for t in writes:
            t.w = [tok]
            t.r = []

    def op(self, engname, fn, reads=(), writes=()):
        return self.group(engname, [fn], reads, writes)

    def serial(self, engname, fns, reads=(), writes=(), rgs=None):
        E = self.E[engname]
        if rgs is None:
            rgs = list(range(len(fns)))
        tok, i = None, 0
        while i < len(fns):
            j = i
            while j + 1 < len(fns) and rgs[j + 1] == rgs[i]:
                j += 1
            if E.cnt > 0:
                self._wait(E, E.sem, E.cnt)
            tok = self.group(engname, fns[i:j + 1], reads, writes)
            i = j + 1
        return tok

    def group(self, engname, fns, reads=(), writes=()):
        E = self.E[engname]
        self._deps(E, reads, writes)
        tok = self._token(E)
        n = len(fns)
        for i, fn in enumerate(fns):
            ins = fn(E.eng)
            E.n_ins += 1
            if i == n - 1:
                ins.then_inc(tok[0], 1)
        self._mark(tok, reads, writes)
        return tok

    def dma(self, qname, out_ap, in_ap, reads=(), writes=(), **kw):
        E = self.E[qname]
        self._deps(E, reads, writes, skip_own_dma=True)
        owner = writes[0] if writes else reads[0]
        if owner.dsem is None:
            owner.dsem = self.new_sem('d_' + owner.name)
        owner.dcnt += 16
        tok = (owner.dsem, owner.dcnt, None)
        E.eng.dma_start(out=out_ap, in_=in_ap, **kw).then_inc(owner.dsem, 16)
        E.n_ins += 1
        self.dma_toks[id(owner.dsem)] = (owner.dsem, owner.dcnt)
        self._mark(tok, reads, writes)
        return tok

    def barrier(self):
        for E in self.E.values():
            for F in self.E.values():
                if F is not E and F.cnt > 0:
                    self._wait(E, F.sem, F.cnt)
            for sem, val in self.dma_toks.values():
                self._wait(E, sem, val)


def build_all(nc, D, ctxP):
    kb = KB(nc, ctxP)
    for i in range(8):
        kb.banks.append(T(kb, f'pb{i}', [128, 512], F32, 'ps'))
    nb = kb.nb

    def mm(out, lhsT, rhs, start, stop):
        return lambda e: e.matmul(out, lhsT=lhsT, rhs=rhs, start=start, stop=stop)

    def tp(out, in_, idn):
        return lambda e: e.transpose(out, in_, idn)

    def tt(out, in0, in1, op):
        return lambda e: e.tensor_tensor(out=out, in0=in0, in1=in1, op=op)

    def ts(out, in0, s1, s2, op0, op1=None):
        if op1 is None:
            return lambda e: e.tensor_scalar(out=out, in0=in0, scalar1=s1, scalar2=None, op0=op0)
        return lambda e: e.tensor_scalar(out=out, in0=in0, scalar1=s1, scalar2=s2, op0=op0, op1=op1)

    def stt(out, in0, sc_, in1, op0, op1):
        return lambda e: e.scalar_tensor_tensor(out=out, in0=in0, scalar=sc_, in1=in1, op0=op0, op1=op1)

    def act(out, in_, func, bias=None, scale=None, accum_out=None):
        kw = {}
        if bias is not None:
            kw['bias'] = bias
        if scale is not None:
            kw['scale'] = scale
        if accum_out is not None:
            kw['accum_out'] = accum_out
        return lambda e: e.activation(out=out, in_=in_, func=func, **kw)

    def cpy(out, in_):
        return lambda e: e.tensor_copy(out=out, in_=in_)

    def mset(ap, v):
        return lambda e: e.memset(ap, v)

    def rcp(out, in_):
        return lambda e: e.reciprocal(out=out, in_=in_)

    def rsum(out, in_):
        return lambda e: e.reduce_sum(out=out, in_=in_, axis=AX.X)

    cp = kb.tile('cp', [128, NCP])
    kb.dma('sp', cp[:], D['cp'], writes=[cp])
    cst = kb.tile('cst', [128, 8])
    for i, v in enumerate([1e-6, 1e-12, 64e-5, 1e-5]):
        kb.op('pool', mset(cst[:, i:i + 1], v), writes=[cst])
    ident = kb.tile('ident', [128, 128])
    kb.op('pool', mset(ident[:], 1.0), writes=[ident])
    kb.op('pool', lambda e: e.affine_select(out=ident[:], in_=ident[:], pattern=[[-1, 128]], compare_op=OP.is_equal,
                                            fill=0.0, base=0, channel_multiplier=1), reads=[ident], writes=[ident])
    ones = kb.tile('ones', [128, 128])
    kb.op('pool', mset(ones[:], 1.0), writes=[ones])
    identb = kb.tile('identb', [128, 128], SDT)
    kb.op('pool', cpy(identb[:], ident[:]), reads=[ident], writes=[identb])
    sc = kb.tile('sc', [128, 8, 2])
    kb.dma('sp', sc[:], D['ccT'], writes=[sc])
    kb.op('act', act(sc[:, :, :], sc[:, :, :], AF.Silu), reads=[sc], writes=[sc])
    modT = kb.tile('modT', [128, 48, 2])
    mv = kb.tile('mv', [128, 6, 8])
    GA = kb.tile('GA', [128, 2048])
    xts = [kb.tile(f'xt{i}', [128, 1024]) for i in range(2)]
    xs = kb.tile('xs', [128, 1024])
    sts = [kb.tile(f'st{i}', [128, 4]) for i in range(2)]
    rot = {'x': 0}

    def emit_hT(src_ap, ntok, vs, vb, dst, src_tile=None):
        i = rot['x'] % 2
        rot['x'] += 1
        st = sts[i]
        if src_tile is None:
            xt = xts[i]
            kb.dma('sp', xt[0:ntok, :], src_ap, writes=[xt])
        else:
            xt = src_tile
        kb.op('act', act(xs[0:ntok, :], xt[0:ntok, :], AF.Square, accum_out=st[0:ntok, 0:1]), reads=[xt], writes=[xs, st])
        kb.op('act', act(st[0:ntok, 1:2], st[0:ntok, 0:1], AF.Sqrt, bias=cst[0:ntok, 0:1], scale=1.0 / 1024),
              reads=[st, cst], writes=[st])
        kb.op('dve', rcp(st[0:ntok, 2:3], st[0:ntok, 1:2]), reads=[st], writes=[st])
        kb.op('dve', ts(xs[0:ntok, :], xt[0:ntok, :], st[0:ntok, 2:3], None, OP.mult), reads=[xt, st], writes=[xs])
        kpb = 512 // ntok
        for g in range(8 // kpb):
            pb = nb()
            kb.group('pe', [tp(pb[:, i2 * ntok:(i2 + 1) * ntok], xs[0:ntok, (g * kpb + i2) * 128:(g * kpb + i2 + 1) * 128],
                               ident[0:ntok, 0:ntok]) for i2 in range(kpb)], reads=[xs, ident], writes=[pb])
            for i2 in range(kpb):
                k = g * kpb + i2
                kb.op('act', act(dst[:, k, 0:ntok], pb[:, i2 * ntok:(i2 + 1) * ntok], AF.Identity,
                                 bias=mv[:, vb, k:k + 1], scale=mv[:, vs, k:k + 1]), reads=[pb, mv], writes=[dst])
        return xt

    with ExitStack() as cA:
        kb.ctx = cA
        was = [kb.tile(f'wa{i}', [128, 8, 256]) for i in range(2)]
        psA = nb()
        for jb in range(24):
            wa = was[jb % 2]
            kb.dma('sp', wa[:], D['w_ada'][:, jb * 256:(jb + 1) * 256].rearrange("(k p) n -> p k n", p=128), writes=[wa])
            fns = []
            for jj in range(2):
                j = jb * 2 + jj
                for k in range(8):
                    fns.append(mm(psA[:, j * 2:(j + 1) * 2], wa[:, k, jj * 128:(jj + 1) * 128], sc[:, k, :], k == 0, k == 7))
            kb.group('pe', fns, reads=[wa, sc], writes=[psA])
        kb.op('dve', tt(modT[:, :, :], psA[:, 0:96].rearrange("p (j n) -> p j n", n=2),
                        cp[:, CP_B48:CP_B48 + 48].unsqueeze(2).to_broadcast([128, 48, 2]), OP.add), reads=[psA, cp], writes=[modT])
        kb.op('dve', stt(mv[:, 0, :], modT[:, 8:16, 0], 1.0, cp[:, CP_G1:CP_G1 + 8], OP.add, OP.mult), reads=[modT, cp], writes=[mv])
        kb.op('dve', cpy(mv[:, 1, :], modT[:, 0:8, 0]), reads=[modT], writes=[mv])
        kb.op('dve', stt(mv[:, 2, :], modT[:, 8:16, 1], 1.0, cp[:, CP_G1:CP_G1 + 8], OP.add, OP.mult), reads=[modT, cp], writes=[mv])
        kb.op('dve', cpy(mv[:, 3, :], modT[:, 0:8, 1]), reads=[modT], writes=[mv])
        kb.op('dve', stt(mv[:, 4, :], modT[:, 32:40, 0], 1.0, cp[:, CP_G2:CP_G2 + 8], OP.add, OP.mult), reads=[modT, cp], writes=[mv])
        kb.op('dve', cpy(mv[:, 5, :], modT[:, 24:32, 0]), reads=[modT], writes=[mv])
        kb.barrier()
    if STOP == 'A':
        kb.dma('sp', D['y'][0:128, 0:48], mv[:, :, :].rearrange("p a b -> p (a b)"), reads=[mv])
        kb.dma('sp', D['y'][0:128, 64:160], modT[:, :, :].rearrange("p a b -> p (a b)"), reads=[modT])
        kb.barrier()
        return

    with ExitStack() as cZ:
        kb.ctx = cZ
        zaT = kb.tile('zaT', [128, 4, NOWN * 64], BF16)
        with ExitStack() as c1:
            kb.ctx = c1
            phase_scan(kb, D, locals())
            kb.barrier()
            if STOP in ('B', 'B0', 'B1', 'C', 'D'):
                return
        with ExitStack() as cE:
            kb.ctx = cE
            phase_merge(kb, D, locals())
            kb.barrier()
            if STOP == 'E':
                return
    with ExitStack() as cF:
        kb.ctx = cF
        phase_moe(kb, D, locals())
        kb.barrier()
    print({k: v.n_ins for k, v in kb.E.items()}, 'sems', kb.nsem)


def phase_scan(kb, D, L):
    nb = kb.nb
    mm, tp, tt, ts, stt, act, cpy, mset, rcp, rsum = (L[k] for k in ('mm', 'tp', 'tt', 'ts', 'stt', 'act', 'cpy', 'mset', 'rcp', 'rsum'))
    cp, cst, ident, ones, mv, zaT, emit_hT = (L[k] for k in ('cp', 'cst', 'ident', 'ones', 'mv', 'zaT', 'emit_hT'))
    identb = L['identb']
    rcg = kb.tile('rcg', [64, 1024])
    kb.dma('sp', rcg[:], D['rcg'], writes=[rcg])
    bdm = kb.tile('bdm', [128, 128])
    kb.op('pool', mset(bdm[:], 0.0), writes=[bdm])
    kb.op('pool', mset(bdm[0:64, 0:64], 1.0), writes=[bdm])
    kb.op('pool', mset(bdm[64:128, 64:128], 1.0), writes=[bdm])
    hsel = kb.tile('hsel', [128, 2])
    kb.op('pool', mset(hsel[:], 0.0), writes=[hsel])
    kb.op('pool', mset(hsel[0:64, 0:1], 1.0), writes=[hsel])
    kb.op('pool', mset(hsel[64:128, 1:2], 1.0), writes=[hsel])
    tri = kb.tile('tri', [64, 4, 64])
    kb.op('pool', mset(tri[:], 1.0), writes=[tri])
    for i, (pat, cm, cmp_) in enumerate([(1, -1, OP.is_gt), (1, -1, OP.is_ge), (-1, 1, OP.is_gt), (-1, 1, OP.is_ge)]):
        kb.op('pool', lambda e, i=i, pat=pat, cm=cm, cmp_=cmp_: e.affine_select(
            out=tri[:, i, :], in_=tri[:, i, :], pattern=[[pat, 64]], compare_op=cmp_, fill=0.0, base=0,
            channel_multiplier=cm), reads=[tri], writes=[tri])
    MT, ML = [], []
    for d in range(2):
        mt = kb.tile(f'mt{d}', [64, 4, 2, 64])
        ml = kb.tile(f'ml{d}', [64, 8, 64])
        s_i, i_i, l_i = (0, 1, 2) if d == 0 else (2, 3, 0)
        kb.op('pool', cpy(mt[:, :, 0, :], tri[:, s_i, :].unsqueeze(1).to_broadcast([64, 4, 64])), reads=[tri], writes=[mt])
        kb.op('pool', cpy(mt[:, :, 1, :], tri[:, i_i, :].unsqueeze(1).to_broadcast([64, 4, 64])), reads=[tri], writes=[mt])
        kb.op('pool', cpy(ml[:, :, :], tri[:, l_i, :].unsqueeze(1).to_broadcast([64, 8, 64])), reads=[tri], writes=[ml])
        MT.append(mt)
        ML.append(ml)
    cf = kb.tile('cf', [128, 8, 15])
    muap = cp[:, CP_MU:CP_MU + 15]
    kb.op('dve', ts(cf[:, 0, :], muap, -1.0, 1.0, OP.mult, OP.add), reads=[cp], writes=[cf])
    for i, off in enumerate((CP_ML, CP_MR, CP_MUP, CP_MDN, CP_CP, CP_CN)):
        kb.op('dve', tt(cf[:, 1 + i, :], muap, cp[:, off:off + 15], OP.mult), reads=[cp], writes=[cf])
    kb.op('dve', ts(cf[:, 7, 0:4], cp[:, CP_KA:CP_KA + 4], -1.0, 1.0, OP.mult, OP.add), reads=[cp], writes=[cf])

    def bc(ap, n, w=64):
        return ap.unsqueeze(2).to_broadcast([128, n, w])

    wr = kb.tile('wr', [128, 8, 1920], BF16)
    kb.dma('pool', wr[:], D['w_in'][:, 0:1920].rearrange("(k p) n -> p k n", p=128), writes=[wr])
    dl = kb.tile('dl', [128, 512])
    il = kb.tile('il', [128, 512])
    gl = kb.tile('gl', [128, 512])
    kb.dma('sp', dl[:], D['dl'], writes=[dl])
    kb.dma('sp', il[:], D['il'], writes=[il])
    kb.dma('sp', gl[:], D['gl'], writes=[gl])

    ST = [kb.tile(f'ST{d}', [128, 4, 64]) for d in range(2)]
    STb = [kb.tile(f'STb{d}', [128, 4, 64], SDT) for d in range(2)]
    for d in range(2):
        kb.op('pool', mset(ST[d][:], 0.0), writes=[ST[d]])
        kb.op('pool', mset(STb[d][:], 0.0), writes=[STb[d]])
    bonL = kb.tile('bonL', [64, NOWN, 8])
    yld = [Sub(None, D['y'][i * 64:(i + 1) * 64, 0:512], f'yld{i}') for i in range(NOWN)]
    Pb = [kb.tile(f'Pb{i}', [128, 15, 64]) for i in range(3)]
    hTs = [kb.tile(f'hT{i}', [128, 8, 64], BF16) for i in range(2)]
    Qs = [kb.tile(f'Q{i}', [128, 15, 64]) for i in range(2)]
    tmpx = [kb.tile(f'tmpx{i}', [128, 15, 64]) for i in range(2)]

    def W4(name):
        return kb.tile(name, [128, 4, 64])
    kk_t, sq_t, kkn_t, sg_t, cum_t, A_t, kd_t, bd_t, t4_t = [W4(n) for n in ('kk', 'sq', 'kkn', 'sg', 'cum', 'A', 'kd', 'bd', 't4')]
    E1 = W4('E1')
    BTt = kb.tile('BT', [128, 5, 64], SDT)
    KTt = kb.tile('KT', [128, 5, 64], SDT)
    BTf = kb.tile('BTf', [128, 5, 64])
    KTf = kb.tile('KTf', [128, 5, 64])
    kb.op('pool', mset(BTf[:], 0.0), writes=[BTf])
    kb.op('pool', mset(KTf[:], 0.0), writes=[KTf])
    kb.op('pool', mset(BTt[:], 0.0), writes=[BTt])
    kb.op('pool', mset(KTt[:], 0.0), writes=[KTt])
    rinv_t, cex_t = sq_t, sq_t
    E3 = kk_t
    E2 = sg_t
    rkr_t = A_t
    AR = kb.tile('AR', [128, 4, 2, 64], SDT)
    th_t = kb.tile('th', [128, 64])
    sgd_t = kb.tile('sgd', [128, 64])
    Vt, Kt, Bt = [kb.tile(n, [64, 512], SDT) for n in ('Vt', 'Kt', 'Bt')]
    SA = kb.tile('SA', [64, 8, 2, 64], SDT)
    SB = kb.tile('SB', [64, 8, 2, 64], SDT)
    Xs = [kb.tile(f'Xl{i}', [64, 8, 64], SDT) for i in range(2)]
    Ys = [kb.tile(f'Yl{i}', [64, 8, 64], SDT) for i in range(2)]
    Us = [kb.tile(f'U{i}', [64, 512], SDT) for i in range(2)]
    bon_t = kb.tile('bon', [64, 8])
    G_t = kb.tile('G', [64, 512])
    y_t = kb.tile('y', [64, 8, 64])
    ysq = kb.tile('ysq', [64, 8, 64])
    yst = kb.tile('yst', [64, 512])
    gst = kb.tile('gst', [64, 6, 8])
    za_t = kb.tile('za', [64, 512])

    def proj(j):
        hT = hTs[j % 2]
        emit_hT(D['xo'][j * 64:(j + 1) * 64, :], 64, 0, 1, hT)
        pbA, pbB = nb(), nb()
        kb.group('pe', [mm(pbA[:, c * 64:(c + 1) * 64], wr[:, k, c * 128:(c + 1) * 128], hT[:, k, 0:64], k == 0, k == 7)
                        for c in range(8) for k in range(8)], reads=[wr, hT], writes=[pbA])
        kb.group('pe', [mm(pbB[:, (c - 8) * 64:(c - 7) * 64], wr[:, k, c * 128:(c + 1) * 128], hT[:, k, 0:64], k == 0, k == 7)
                        for c in range(8, 15) for k in range(8)], reads=[wr, hT], writes=[pbB])
        P = Pb[j % 3]
        kb.op('act', act(P[:, 0:8, :], pbA[:, 0:512].rearrange("p (c t) -> p c t", t=64), AF.Copy), reads=[pbA], writes=[P])
        kb.op('dve', cpy(P[:, 8:15, :], pbB[:, 0:448].rearrange("p (c t) -> p c t", t=64)), reads=[pbB], writes=[P])

    def lerp(j, has_prev, has_next):
        P = Pb[j % 3]
        Q = Qs[j % 2]
        t1, t2 = tmpx
        kb.op('dve', tt(Q[:, :, :], P[:, :, :], bc(cf[:, 0, :], 15), OP.mult), reads=[cf, P], writes=[Q])
        kb.op('pool', tt(t1[:, :, 1:64], P[:, :, 0:63], bc(cf[:, 1, :], 15, 63), OP.mult), reads=[cf, P], writes=[t1])
        kb.op('dve', tt(Q[:, :, 1:64], Q[:, :, 1:64], t1[:, :, 1:64], OP.add), reads=[Q, t1], writes=[Q])
        kb.op('pool', tt(t2[:, :, 0:63], P[:, :, 1:64], bc(cf[:, 2, :], 15, 63), OP.mult), reads=[cf, P], writes=[t2])
        kb.op('dve', tt(Q[:, :, 0:63], Q[:, :, 0:63], t2[:, :, 0:63], OP.add), reads=[Q, t2], writes=[Q])
        if has_prev:
            Pp = Pb[(j - 1) % 3]
            kb.op('pool', tt(t1[:, :, :], Pp[:, :, :], bc(cf[:, 3, :], 15), OP.mult), reads=[cf, Pp], writes=[t1])
            kb.op('dve', tt(Q[:, :, :], Q[:, :, :], t1[:, :, :], OP.add), reads=[Q, t1], writes=[Q])
        if has_next:
            Pn = Pb[(j + 1) % 3]
            kb.op('pool', tt(t2[:, :, :], Pn[:, :, :], bc(cf[:, 4, :], 15), OP.mult), reads=[cf, Pn], writes=[t2])
            kb.op('dve', tt(Q[:, :, :], Q[:, :, :], t2[:, :, :], OP.add), reads=[Q, t2], writes=[Q])
        return Q

    hp = [slice(0, 64), slice(64, 128)]
    HORD = (0, 2, 4, 6, 1, 3, 5, 7)

    def chunk_step(Q, qv, d, emit, own_idx, second):
        dsl = hp[d]
        last = 63 if d == 0 else 0
        kb.op('pool', tt(kk_t[:, :, :], qv(4, 8), bc(cp[:, CP_KK:CP_KK + 4], 4), OP.mult), reads=[Q, cp], writes=[kk_t])
        kb.op('pool', tt(sq_t[:, :, :], kk_t[:, :, :], kk_t[:, :, :], OP.mult), reads=[kk_t], writes=[sq_t])
        pm = nb()
        kb.group('pe', [mm(pm[:, c * 64:(c + 1) * 64], bdm[:, :], sq_t[:, c, :], True, True) for c in range(4)],
                 reads=[bdm, sq_t], writes=[pm])
        kb.op('act', act(rinv_t[:, :, :], pm[:, 0:256].rearrange("p (c t) -> p c t", t=64), AF.Sqrt, bias=cst[:, 1:2]),
              reads=[pm, cst], writes=[rinv_t])
        kb.op('dve', rcp(rinv_t[:, :, :], rinv_t[:, :, :]), reads=[rinv_t], writes=[rinv_t])
        kb.op('dve', tt(kkn_t[:, :, :], kk_t[:, :, :], rinv_t[:, :, :], OP.mult), reads=[kk_t, rinv_t], writes=[kkn_t])
        ck(1)
        kb.op('act', act(th_t[dsl, :], qv(12, 13)[dsl, 0, :], AF.Tanh), reads=[Q], writes=[th_t])
        pw = nb()
        kb.group('pe', [mm(pw[:, c * 64:(c + 1) * 64], dl[dsl, c * 128:(c + 1) * 128], th_t[dsl, :], True, True) for c in range(4)]
                 + [mm(pw[:, 256 + c * 64:256 + (c + 1) * 64], il[dsl, c * 128:(c + 1) * 128], qv(13, 14)[dsl, 0, :], True, True)
                    for c in range(4)], reads=[dl, il, th_t, Q], writes=[pw])
        for c in range(4):
            kb.op('act', act(sg_t[:, c, :], pw[:, c * 64:(c + 1) * 64], AF.Sigmoid,
                             bias=cp[:, CP_W0 + d * 4 + c:CP_W0 + d * 4 + c + 1]), reads=[pw, cp], writes=[sg_t])
        for c in range(4):
            kb.op('act', act(A_t[:, c, :], pw[:, 256 + c * 64:256 + (c + 1) * 64], AF.Sigmoid,
                             bias=cp[:, CP_A0 + d * 4 + c:CP_A0 + d * 4 + c + 1]), reads=[pw, cp], writes=[A_t])
        ck(2)
        for c in range(4):
            kb.op('dve', lambda e, c=c: e.tensor_tensor_scan(out=cum_t[:, c, :], data0=ones[:, 0:64], data1=sg_t[:, c, :],
                                                             initial=0.0, op0=OP.mult, op1=OP.add), reads=[ones, sg_t], writes=[cum_t])
        if d == 1:
            kb.op('dve', tt(cex_t[:, :, :], sg_t[:, :, :], cum_t[:, :, :], OP.subtract), reads=[sg_t, cum_t], writes=[cex_t])
            kb.op('dve', tt(t4_t[:, :, :], cex_t[:, :, :], cum_t[:, :, 63:64].to_broadcast([128, 4, 64]), OP.add),
                  reads=[cex_t, cum_t], writes=[t4_t])
            kb.op('dve', cpy(cum_t[:, :, :], t4_t[:, :, :]), reads=[t4_t], writes=[cum_t])
        kb.op('dve', tt(cex_t[:, :, :], cum_t[:, :, :], sg_t[:, :, :], OP.subtract), reads=[cum_t, sg_t], writes=[cex_t])
        kb.op('pool', tt(t4_t[:, :, :], A_t[:, :, :], bc(cp[:, CP_KA:CP_KA + 4], 4), OP.mult), reads=[A_t, cp], writes=[t4_t])
        kb.op('pool', tt(t4_t[:, :, :], t4_t[:, :, :], bc(cf[:, 7, 0:4], 4), OP.add), reads=[t4_t, cf], writes=[t4_t])
        kb.op('pool', tt(kd_t[:, :, :], qv(4, 8), t4_t[:, :, :], OP.mult), reads=[Q, t4_t], writes=[kd_t])
        kb.op('dve', tt(bd_t[:, :, :], kkn_t[:, :, :], A_t[:, :, :], OP.mult), reads=[kkn_t, A_t], writes=[bd_t])
        kb.op('act', act(E1[:, :, :], cum_t[:, :, :], AF.Exp, scale=-C0), reads=[cum_t], writes=[E1])
        kb.op('act', act(E3[:, :, :], cex_t[:, :, :], AF.Exp, scale=-C0), reads=[cex_t], writes=[E3])
        kb.op('act', act(E2[:, :, :], cum_t[:, :, :], AF.Exp, scale=C0), reads=[cum_t], writes=[E2])
        kb.op('dve', stt(AR[:, :, 0, :], kkn_t[:, :, :], -1.0, E3[:, :, :], OP.mult, OP.mult), reads=[kkn_t, E3], writes=[AR])
        kb.op('dve', tt(AR[:, :, 1, :], qv(0, 4), E1[:, :, :], OP.mult), reads=[Q, E1], writes=[AR])
        kb.op('pool', tt(BTt[:, 0:4, :], bd_t[:, :, :], E2[:, :, :], OP.mult), reads=[bd_t, E2], writes=[BTt])
        kb.op('pool', tt(KTt[:, 0:4, :], kd_t[:, :, :], E2[:, :, :], OP.mult), reads=[kd_t, E2], writes=[KTt])
        ck(3)
        kb.op('pool', tt(BTf[:, 0:4, :], bd_t[:, :, :], E2[:, :, :], OP.mult), reads=[bd_t, E2], writes=[BTf])
        kb.op('pool', tt(KTf[:, 0:4, :], kd_t[:, :, :], E2[:, :, :], OP.mult), reads=[kd_t, E2], writes=[KTf])
        for srcAP, dst, tracked in ((lambda c: qv(8 + c, 10 + c), Vt, Q), (lambda c: KTf[:, c:c + 2, :], Kt, KTf),
                                    (lambda c: BTf[:, c:c + 2, :], Bt, BTf)):
            pt = nb()
            kb.group('pe', [tp(pt[:, c * 128:(c + 1) * 128], srcAP(c), ident[:, :]) for c in range(4)],
                     reads=[tracked, ident], writes=[pt])
            kb.op('act', act(dst[:, :], pt[0:64, :], AF.Copy), reads=[pt], writes=[dst])
        if emit:
            kb.op('pool', tt(rkr_t[:, :, :], qv(0, 4), kd_t[:, :, :], OP.mult), reads=[Q, kd_t], writes=[rkr_t])
            kb.op('pool', tt(rkr_t[:, :, :], rkr_t[:, :, :], bc(cp[:, CP_RK:CP_RK + 4], 4), OP.mult), reads=[rkr_t, cp], writes=[rkr_t])
            pbn = nb()
            kb.group('pe', [mm(pbn[0:64, c * 2:(c + 1) * 2], rkr_t[:, c, :], hsel[:, :], True, True) for c in range(4)],
                     reads=[rkr_t, hsel], writes=[pbn])
            if not second:
                kb.op('dve', cpy(bonL[:, own_idx, :], pbn[0:64, 0:8]), reads=[pbn], writes=[bonL])
            else:
                kb.op('dve', tt(bon_t[:, :], pbn[0:64, 0:8], bonL[:, own_idx, :], OP.add), reads=[pbn, bonL], writes=[bon_t])
                kb.op('act', act(sgd_t[:, :], qv(14, 15)[:, 0, :], AF.Sigmoid), reads=[Q], writes=[sgd_t])
                pgt = nb()
                kb.op('pe', mm(pgt[0:64, :], sgd_t[:, :], gl[:, :], True, True), reads=[sgd_t, gl], writes=[pgt])
                kb.op('act', act(G_t[:, :], pgt[0:64, :], AF.Copy), reads=[pgt], writes=[G_t])
                kb.dma('sp', yst[:, :], yld[own_idx][:, :], reads=[yld[own_idx]], writes=[yst])
        ck(4)
        S = ST[d]
        Sb = STb[d]
        for src, dstS in ((BTt, SA), (KTt, SB)):
            for g in range(2):
                pa = nb()
                kb.serial('pe', [mm(pa[0:64, hh * 128:(hh + 1) * 128], src[hp[(4 * g + hh) % 2], (4 * g + hh) // 2, :],
                                   AR[hp[(4 * g + hh) % 2], (4 * g + hh) // 2, :, :].rearrange("p a t -> p (a t)"), True, True)
                                for hh in (0, 2, 1, 3)], reads=[src, AR], writes=[pa], rgs=[0, 0, 1, 1])
                kb.op('dve', tt(dstS[:, 4 * g:4 * g + 4, :, :], pa[0:64, :].rearrange("p (h a t) -> p h a t", a=2, t=64),
                                MT[d][:, :, :, :], OP.mult), reads=[pa, MT[d]], writes=[dstS])
        pl = nb()
        kb.serial('pe', [mm(pl[0:64, h * 64:(h + 1) * 64], AR[hp[h % 2], h // 2, 0, :], BTt[hp[h % 2], h // 2, :], True, True)
                        for h in HORD], reads=[AR, BTt], writes=[pl], rgs=[h % 2 for h in HORD])
        kb.op('dve', tt(Ys[0][:, :, :], pl[0:64, :].rearrange("p (h t) -> p h t", t=64), ML[d][:, :, :], OP.mult),
              reads=[pl, ML[d]], writes=[Ys[0]])
        ck(5)
        pwu = nb()
        fns, rgs = [], []
        for h in HORD:
            fns.append(mm(pwu[0:64, h * 64:(h + 1) * 64], AR[hp[h % 2], h // 2, 0, :], Sb[hp[h % 2], h // 2, :], True, False))
            fns.append(mm(pwu[0:64, h * 64:(h + 1) * 64], SB[:, h, 0, :], Vt[:, h * 64:(h + 1) * 64], False, True))
            rgs += [h % 2, 0]
        kb.serial('pe', fns, reads=[AR, Sb, SB, Vt], writes=[pwu], rgs=rgs)
        U = Us[0]
        kb.op('act', act(U[:, :], pwu[0:64, :], AF.Copy), reads=[pwu], writes=[U])
        XT_ap = lambda h: SA[:, h, 0, :]
        Xtile = SA
        Ycur = Ys[0]
        ui = 0
        for n in range(6):
            pu = nb()
            kb.group('pe', [mm(pu[0:64, h * 64:(h + 1) * 64], XT_ap(h), U[:, h * 64:(h + 1) * 64], True, True) for h in range(8)],
                     reads=[Xtile, U], writes=[pu])
            Un = Us[(ui + 1) % 2]
            kb.op('dve', tt(Un[:, :], pu[0:64, :], U[:, :], OP.add), reads=[pu, U], writes=[Un])
            if n < 5:
                Xn = Xs[n % 2]
                px = nb()
                kb.group('pe', [mm(px[0:64, h * 64:(h + 1) * 64], Ycur[:, h, :], XT_ap(h), True, True) for h in range(8)],
                         reads=[Ycur, Xtile], writes=[px])
                kb.op('act', act(Xn[:, :, :], px[0:64, :].rearrange("p (h t) -> p h t", t=64), AF.Copy), reads=[px], writes=[Xn])
                if n < 4:
                    Yn = Ys[(n + 1) % 2]
                    py = nb()
                    kb.group('pe', [mm(py[0:64, h * 64:(h + 1) * 64], XT_ap(h), Ycur[:, h, :], True, True) for h in range(8)],
                             reads=[Ycur, Xtile], writes=[py])
                    kb.op('act', act(Yn[:, :, :], py[0:64, :].rearrange("p (h t) -> p h t", t=64), AF.Copy), reads=[py], writes=[Yn])
                    Ycur = Yn
                Xtile = Xn
                XT_ap = (lambda Xn_: (lambda h: Xn_[:, h, :]))(Xn)
            U = Un
            ui += 1
        ck(6)
        if emit:
            pyo = nb()
            fns, rgs = [], []
            for h in HORD:
                o = pyo[0:64, h * 64:(h + 1) * 64]
                fns.append(mm(o, AR[hp[h % 2], h // 2, 1, :], Sb[hp[h % 2], h // 2, :], True, False))
                fns.append(mm(o, SA[:, h, 1, :], U[:, h * 64:(h + 1) * 64], False, False))
                fns.append(mm(o, SB[:, h, 1, :], Vt[:, h * 64:(h + 1) * 64], False, True))
                rgs += [h % 2, 0, 0]
            kb.serial('pe', fns, reads=[AR, Sb, SA, SB, U, Vt], writes=[pyo], rgs=rgs)
            if not second:
                kb.op('act', act(yst[:, :], pyo[0:64, :], AF.Copy), reads=[pyo], writes=[yst])
                kb.dma('sp', yld[own_idx][:, :], yst[:, :], reads=[yst], writes=[yld[own_idx]])
            else:
                kb.op('dve', tt(y_t[:, :, :], pyo[0:64, :].rearrange("p (h t) -> p h t", t=64),
                                yst[:, :].rearrange("p (h t) -> p h t", t=64), OP.add), reads=[pyo, yst], writes=[y_t])
        ps_ = nb()
        fns = []
        for h in range(8):
            c = h // 2
            o = ps_[:, h * 64:(h + 1) * 64]
            fns.append(mm(o, Bt[:, c * 128:(c + 1) * 128], U[:, h * 64:(h + 1) * 64], True, False))
            fns.append(mm(o, Kt[:, c * 128:(c + 1) * 128], Vt[:, h * 64:(h + 1) * 64], False, True))
        kb.group('pe', fns, reads=[Bt, Kt, U, Vt], writes=[ps_])
        psv = ps_[:, :].rearrange("p (c q t) -> p c q t", q=2, t=64)
        for q in range(2):
            kb.op('dve', tt(t4_t[hp[q], :, :], psv[hp[q], :, q, :], S[hp[q], :, :], OP.add), reads=[ps_, S], writes=[t4_t])
        kb.op('dve', tt(S[:, :, :], t4_t[:, :, :], E1[:, :, last:last + 1].to_broadcast([128, 4, 64]), OP.mult),
              reads=[t4_t, E1], writes=[S])
        kb.op('pool', cpy(Sb[:, :, :], S[:, :, :]), reads=[S], writes=[Sb])
        if emit and second:
            kb.op('dve', rsum(gst[:, 0, :], y_t[:, :, :]), reads=[y_t], writes=[gst])
            kb.op('pool', tt(ysq[:, :, :], y_t[:, :, :], y_t[:, :, :], OP.mult), reads=[y_t], writes=[ysq])
            kb.op('dve', rsum(gst[:, 1, :], ysq[:, :, :]), reads=[ysq], writes=[gst])
            kb.op('dve', ts(gst[:, 2, :], gst[:, 0, :], 1.0 / 64, None, OP.mult), reads=[gst], writes=[gst])
            kb.op('dve', tt(gst[:, 3, :], gst[:, 2, :], gst[:, 2, :], OP.mult), reads=[gst], writes=[gst])
            kb.op('dve', stt(gst[:, 4, :], gst[:, 1, :], 1.0 / 64, gst[:, 3, :], OP.mult, OP.subtract), reads=[gst], writes=[gst])
            kb.op('act', act(gst[:, 5, :], gst[:, 4, :], AF.Sqrt, bias=cst[0:64, 2:3]), reads=[gst, cst], writes=[gst])
            kb.op('dve', rcp(gst[:, 5, :], gst[:, 5, :]), reads=[gst], writes=[gst])
            kb.op('dve', tt(y_t[:, :, :], y_t[:, :, :], gst[:, 2, :].unsqueeze(2).to_broadcast([64, 8, 64]), OP.subtract),
                  reads=[y_t, gst], writes=[y_t])
            kb.op('dve', tt(y_t[:, :, :], y_t[:, :, :], gst[:, 5, :].unsqueeze(2).to_broadcast([64, 8, 64]), OP.mult),
                  reads=[y_t, gst], writes=[y_t])
            yf = y_t[:, :, :].rearrange("p h t -> p (h t)")
            kb.op('pool', tt(yf, yf, rcg[:, 0:512], OP.mult), reads=[y_t, rcg], writes=[y_t])
            kb.op('pool', tt(yf, yf, rcg[:, 512:1024], OP.add), reads=[y_t, rcg], writes=[y_t])
            kb.op('dve', tt(ysq[:, :, :], Vt[:, :].rearrange("p (h t) -> p h t", t=64),
                            bon_t[:, :].unsqueeze(2).to_broadcast([64, 8, 64]), OP.mult), reads=[Vt, bon_t], writes=[ysq])
            kb.op('dve', tt(y_t[:, :, :], y_t[:, :, :], ysq[:, :, :], OP.add), reads=[y_t, ysq], writes=[y_t])
            kb.op('dve', tt(za_t[:, :], yf, G_t[:, :], OP.mult), reads=[y_t, G_t], writes=[za_t])
            pz = nb()
            kb.group('pe', [tp(pz[:, c * 64:(c + 1) * 64], za_t[:, c * 128:(c + 1) * 128], ident[0:64, 0:64]) for c in range(4)],
                     reads=[za_t, ident], writes=[pz])
            kb.op('act', act(zaT[:, :, own_idx * 64:(own_idx + 1) * 64], pz[:, 0:256].rearrange("p (c t) -> p c t", t=64), AF.Copy),
                  reads=[pz], writes=[zaT])

    with ExitStack() as cB:
        save = kb.ctx
        kb.ctx = cB
        hTc = kb.tile('hTc', [128, 8, 256], BF16)
        Pc = kb.tile('Pc', [128, 15, 256])
        Qc = kb.tile('Qc', [128, 15, 256])
        for i in range(2):
            hsub = Sub(hTc, hTc[:, :, i * 128:(i + 1) * 128], f'hTc{i}')
            emit_hT(D['ctxo'][i * 128:(i + 1) * 128, :], 128, 2, 3, hsub)
            hTc.w = hTc.w + hsub.w
        for c2 in range(8):
            cs = [c for c in (2 * c2, 2 * c2 + 1) if c < 15]
            pb = nb()
            kb.group('pe', [mm(pb[:, i * 256:(i + 1) * 256], wr[:, k, c * 128:(c + 1) * 128], hTc[:, k, :], k == 0, k == 7)
                            for i, c in enumerate(cs) for k in range(8)], reads=[wr, hTc], writes=[pb])
            kb.op('act', act(Pc[:, cs[0]:cs[-1] + 1, :], pb[:, 0:256 * len(cs)].rearrange("p (c t) -> p c t", t=256), AF.Copy),
                  reads=[pb], writes=[Pc])
        for c in range(15):
            kb.op('dve', ts(Qc[:, c, :], Pc[:, c, :], cf[:, 0, c:c + 1], None, OP.mult), reads=[Pc, cf], writes=[Qc])
            kb.op('dve', stt(Qc[:, c, 1:256], Pc[:, c, 0:255], cf[:, 5, c:c + 1], Qc[:, c, 1:256], OP.mult, OP.add),
                  reads=[Pc, cf, Qc], writes=[Qc])
            kb.op('dve', stt(Qc[:, c, 0:255], Pc[:, c, 1:256], cf[:, 6, c:c + 1], Qc[:, c, 0:255], OP.mult, OP.add),
                  reads=[Pc, cf, Qc], writes=[Qc])
        try:
          for d in range(2):
            if STOP == 'B0':
                break
            order = range(NCTXC) if d == 0 else range(NCTXC - 1, -1, -1)
            for j in order:
                Qj = Qs[j % 2]
                kb.op('pool', cpy(Qj[:, :, :], Qc[:, :, j * 64:(j + 1) * 64]), reads=[Qc], writes=[Qj])
                chunk_step(Qj, (lambda Q_: (lambda a, b: Q_[:, a:b, :]))(Qj), d, False, 0, False)
                if STOP == 'B1':
                    raise _Stop()
        except _Stop:
            pass
        if STOP in ('B', 'B0', 'B1'):
            for d in range(2):
                kb.dma('sp', D['y'][0:128, d * 256:(d + 1) * 256], ST[d][:, :, :].rearrange("p a b -> p (a b)"), reads=[ST[d]])
            kb.dma('sp', D['y'][128:256, 0:960], Qc[:, :, 0:64], reads=[Qc])
        kb.barrier()
        kb.ctx = save
    if STOP in ('B', 'B0', 'B1'):
        return

    proj(0)
    for j in range(NCH):
        if j + 1 < NCH:
            proj(j + 1)
        Q = lerp(j, j > 0, j + 1 < NCH)
        own = j - (NCH - NOWN)
        chunk_step(Q, (lambda Q_: (lambda a, b: Q_[:, a:b, :]))(Q), 0, own >= 0, max(own, 0), False)
    lo = NCH - NOWN
    proj(NCH - 1)
    for j in range(NCH - 1, lo - 1, -1):
        if j - 1 >= 0:
            proj(j - 1)
        Q = lerp(j, j - 1 >= 0, j + 1 < NCH)
        chunk_step(Q, (lambda Q_: (lambda a, b: Q_[:, a:b, :]))(Q), 1, True, j - lo, True)


def phase_merge(kb, D, L):
    nb = kb.nb
    mm, tp, tt, ts, stt, act, cpy, mset, rcp, rsum = (L[k] for k in ('mm', 'tp', 'tt', 'ts', 'stt', 'act', 'cpy', 'mset', 'rcp', 'rsum'))
    cp, cst, ident, ones, mv, zaT, emit_hT, GA, sc = (L[k] for k in ('cp', 'cst', 'ident', 'ones', 'mv', 'zaT', 'emit_hT', 'GA', 'sc'))
    NT = NOWN * 64 // 128
    with ExitStack() as c0:
        save = kb.ctx
        kb.ctx = c0
        was = [kb.tile(f'wb{i}', [128, 8, 512]) for i in range(2)]
        garow = kb.tile('garow', [2, 2048])
        bga = kb.tile('bga', [2, 2048])
        kb.dma('sp', bga[:], D['bga'], writes=[bga])
        for i, jb in enumerate((4, 5, 10, 11)):
            wa = was[i % 2]
            kb.dma('sp', wa[:], D['w_ada'][:, jb * 512:(jb + 1) * 512].rearrange("(k p) n -> p k n", p=128), writes=[wa])
            pr = nb()
            kb.group('pe', [mm(pr[0:2, 0:512], sc[:, k, :], wa[:, k, :], k == 0, k == 7) for k in range(8)],
                     reads=[wa, sc], writes=[pr])
            kb.op('dve', tt(garow[0:2, i * 512:(i + 1) * 512], pr[0:2, 0:512], bga[0:2, i * 512:(i + 1) * 512], OP.add),
                  reads=[pr, bga], writes=[garow])
        for q in range(4):
            pg = nb()
            kb.op('pe', mm(pg[:, 0:512], ones[0:1, 0:128], garow[0:1, q * 512:(q + 1) * 512], True, True),
                  reads=[ones, garow], writes=[pg])
            kb.op('act', act(GA[:, q * 512:(q + 1) * 512], pg[:, 0:512], AF.Copy), reads=[pg], writes=[GA])
        kb.barrier()
        kb.ctx = save
    wsg = kb.tile('wsg', [128, 8, 1024], BF16)
    wgt = kb.tile('wgt', [128, 8, 2048], BF16)
    kb.dma('pool', wsg[:], D['w_in'][:, 1920:2944].rearrange("(k p) n -> p k n", p=128), writes=[wsg])
    kb.dma('pool', wgt[:, :, 0:1024], D['w_in'][:, 2944:3968].rearrange("(k p) n -> p k n", p=128), writes=[wgt])
    kb.dma('pool', wgt[:, :, 1024:2048], D['w_in'][:, 3968:4992].rearrange("(k p) n -> p k n", p=128), writes=[wgt])
    worw = kb.tile('worw', [128, 4, 1024], BF16)
    wosg = kb.tile('wosg', [128, 4, 1024], BF16)
    wo = kb.tile('wo', [128, 8, 1024], BF16)
    kb.dma('pool', worw[:], D['w_orw'].rearrange("(k p) n -> p k n", p=128), writes=[worw])
    kb.dma('pool', wosg[:], D['w_osg'].rearrange("(k p) n -> p k n", p=128), writes=[wosg])
    kb.dma('pool', wo[:], D['w_o'].rearrange("(k p) n -> p k n", p=128), writes=[wo])
    wsp = kb.tile('wsp', [128, 8, 128], BF16)
    kb.dma('pool', wsp[:], D['wspT'], writes=[wsp])
    bsp = kb.tile('bsp', [128, 512])
    kb.dma('sp', bsp[:], D['bsp'], writes=[bsp])
    rce = kb.tile('rce', [128, 1024])
    kb.dma('sp', rce[:], D['rce'], writes=[rce])

    hT = kb.tile('hTe', [128, 8, 128], BF16)
    u_t = kb.tile('u', [128, 512])
    z_t = kb.tile('z', [128, 512])
    znb = kb.tile('znb', [128, 512], BF16)
    su = kb.tile('su', [128, 512])
    suT = kb.tile('suT', [128, 4, 128], BF16)
    sga = kb.tile('sga', [128, 512])
    sgb = kb.tile('sgb', [128, 512])
    m_t = kb.tile('m', [128, 1024])
    mT = kb.tile('mT', [128, 8, 128], BF16)
    xn = kb.tile('xn', [128, 1024])
    ls = kb.tile('ls', [128, 8])
    base = (NCH - NOWN) * 64

    for ti in range(NT):
        xt = emit_hT(D['xo'][base + ti * 128:base + (ti + 1) * 128, :], 128, 0, 1, hT)
        for half, dst in ((0, u_t), (1, z_t)):
            pb = nb()
            kb.group('pe', [mm(pb[:, :], hT[:, k, :], wsg[:, k, half * 512:(half + 1) * 512], k == 0, k == 7) for k in range(8)],
                     reads=[hT, wsg], writes=[pb])
            kb.op('act', act(dst[:, :], pb[:, :], AF.Gelu), reads=[pb], writes=[dst])
        kb.op('dve', rsum(ls[:, 0:1], z_t[:, :]), reads=[z_t], writes=[ls])
        kb.op('act', act(su[:, :], z_t[:, :], AF.Square, accum_out=ls[:, 1:2]), reads=[z_t], writes=[su, ls])
        kb.op('dve', ts(ls[:, 2:3], ls[:, 0:1], 1.0 / 512, None, OP.mult), reads=[ls], writes=[ls])
        kb.op('dve', tt(ls[:, 3:4], ls[:, 2:3], ls[:, 2:3], OP.mult), reads=[ls], writes=[ls])
        kb.op('dve', stt(ls[:, 4:5], ls[:, 1:2], 1.0 / 512, ls[:, 3:4], OP.mult, OP.subtract), reads=[ls], writes=[ls])
        kb.op('act', act(ls[:, 5:6], ls[:, 4:5], AF.Sqrt, bias=cst[:, 3:4]), reads=[ls, cst], writes=[ls])
        kb.op('dve', rcp(ls[:, 5:6], ls[:, 5:6]), reads=[ls], writes=[ls])
        kb.op('dve', ts(z_t[:, :], z_t[:, :], ls[:, 2:3], ls[:, 5:6], OP.subtract, OP.mult), reads=[z_t, ls], writes=[z_t])
        kb.op('pool', tt(z_t[:, :], z_t[:, :], rce[:, 0:512], OP.mult), reads=[z_t, rce], writes=[z_t])
        kb.op('pool', tt(znb[:, :], z_t[:, :], rce[:, 512:1024], OP.add), reads=[z_t, rce], writes=[znb])
        pb = nb()
        kb.group('pe', [mm(pb[:, g * 64:(g + 1) * 64], wsp[:, g, :], znb[:, g * 64:(g + 1) * 64], True, True) for g in range(8)],
                 reads=[wsp, znb], writes=[pb])
        kb.op('dve', tt(su[:, :], pb[:, :], bsp[:, :], OP.add), reads=[pb, bsp], writes=[su])
        kb.op('pool', tt(su[:, :], su[:, :], u_t[:, :], OP.mult), reads=[su, u_t], writes=[su])
        pb = nb()
        kb.group('pe', [tp(pb[:, c * 128:(c + 1) * 128], su[:, c * 128:(c + 1) * 128], ident[:, :]) for c in range(4)],
                 reads=[su, ident], writes=[pb])
        kb.op('act', act(suT[:, :, :], pb[:, :].rearrange("p (c t) -> p c t", t=128), AF.Copy), reads=[pb], writes=[suT])
        for half in range(2):
            hs = slice(half * 512, (half + 1) * 512)
            pya, pyb, pga, pgb = nb(), nb(), nb(), nb()
            kb.group('pe', [mm(pya[:, :], zaT[:, c, ti * 128:(ti + 1) * 128], worw[:, c, hs], c == 0, c == 3) for c in range(4)],
                     reads=[zaT, worw], writes=[pya])
            kb.group('pe', [mm(pyb[:, :], suT[:, c, :], wosg[:, c, hs], c == 0, c == 3) for c in range(4)],
                     reads=[suT, wosg], writes=[pyb])
            kb.group('pe', [mm(pga[:, :], hT[:, k, :], wgt[:, k, half * 512:(half + 1) * 512], k == 0, k == 7) for k in range(8)],
                     reads=[hT, wgt], writes=[pga])
            kb.group('pe', [mm(pgb[:, :], hT[:, k, :], wgt[:, k, 1024 + half * 512:1024 + (half + 1) * 512], k == 0, k == 7)
                            for k in range(8)], reads=[hT, wgt], writes=[pgb])
            kb.op('act', act(sga[:, :], pga[:, :], AF.Sigmoid), reads=[pga], writes=[sga])
            kb.op('act', act(sgb[:, :], pgb[:, :], AF.Sigmoid), reads=[pgb], writes=[sgb])
            kb.op('dve', tt(sga[:, :], sga[:, :], pya[:, :], OP.mult), reads=[sga, pya], writes=[sga])
            kb.op('dve', tt(sgb[:, :], sgb[:, :], pyb[:, :], OP.mult), reads=[sgb, pyb], writes=[sgb])
            kb.op('pool', tt(m_t[:, hs], sga[:, :], sgb[:, :], OP.add), reads=[sga, sgb], writes=[m_t])
        for g in range(2):
            pb = nb()
            kb.group('pe', [tp(pb[:, i * 128:(i + 1) * 128], m_t[:, (g * 4 + i) * 128:(g * 4 + i + 1) * 128], ident[:, :])
                            for i in range(4)], reads=[m_t, ident], writes=[pb])
            kb.op('act', act(mT[:, g * 4:(g + 1) * 4, :], pb[:, :].rearrange("p (c t) -> p c t", t=128), AF.Copy), reads=[pb], writes=[mT])
        for half in range(2):
            hs = slice(half * 512, (half + 1) * 512)
            pb = nb()
            kb.group('pe', [mm(pb[:, :], mT[:, k, :], wo[:, k, hs], k == 0, k == 7) for k in range(8)], reads=[mT, wo], writes=[pb])
            kb.op('dve', tt(sga[:, :], pb[:, :], GA[:, hs], OP.mult), reads=[pb, GA], writes=[sga])
            kb.op('pool', tt(xn[:, hs], sga[:, :], xt[:, hs], OP.add), reads=[sga, xt], writes=[xn])
        kb.dma('sp', D['y'][ti * 128:(ti + 1) * 128, :], xn[:, :], reads=[xn])


def phase_moe(kb, D, L):
    nb = kb.nb
    mm, tp, tt, ts, stt, act, cpy, mset, rcp, rsum = (L[k] for k in ('mm', 'tp', 'tt', 'ts', 'stt', 'act', 'cpy', 'mset', 'rcp', 'rsum'))
    cp, cst, ident, ones, mv, emit_hT, GA = (L[k] for k in ('cp', 'cst', 'ident', 'ones', 'mv', 'emit_hT', 'GA'))
    NT = NOWN * 64 // 128
    NH = NT // 2
    rcf = kb.tile('rcf', [128, 1056])
    kb.dma('sp', rcf[:], D['rcf'], writes=[rcf])
    rw = kb.tile('rw', [128, 8, 32])
    kb.dma('sp', rw[:], D['router_w'].rearrange("(k p) n -> p k n", p=128), writes=[rw])
    bgu = kb.tile('bgu', [128, NEXP, 16])
    kb.dma('sp', bgu[:], D['bgu'], writes=[bgu])
    bdt = kb.tile('bdt', [32, 1024])
    kb.dma('sp', bdt[:], D['bd'], writes=[bdt])
    kb.op('dve', tt(bdt[:, :], bdt[:, :], GA[0:32, 1024:2048], OP.mult), reads=[bdt, GA], writes=[bdt])
    acc = kb.tile('acc', [128, NH, 1024])
    h2T = kb.tile('h2T', [128, 8, NH * 128], BF16)
    gw = kb.tile('gw', [128, NH, 32])
    gwT = kb.tile('gwT', [32, NH, 128])
    h2f = kb.tile('h2f', [128, 8, 128])
    lg = kb.tile('lg', [128, 32])
    top8 = kb.tile('top8', [128, 8])
    msk = kb.tile('msk', [128, 32])
    ex = kb.tile('ex', [128, 32])
    fs = kb.tile('fs', [128, 4])
    NW = 5
    wring = [kb.tile(f'wring{i}', [128, 8, 1024], BF16) for i in range(NW)]
    wi = [0]
    WT = {}
    NV = 2 * NEXP

    def wload(v, which):
        if v >= NV:
            return
        t = wring[wi[0] % NW]
        wi[0] += 1
        src = D[{'g': 'wg', 'u': 'wu', 'd': 'wd'}[which]][v % NEXP]
        kb.dma('pool', t[:], src.rearrange("(k p) n -> p k n", p=128), writes=[t])
        WT[(v, which)] = t
    actT = [kb.tile(f'actT{i}', [128, 8, 256], BF16) for i in range(2)]
    gcs = [kb.tile(f'gc{i}', [128, 256]) for i in range(2)]
    u0s = [kb.tile(f'u0{i}', [128, 256]) for i in range(2)]
    sgs = [kb.tile(f'sgm{i}', [128, 256]) for i in range(2)]
    ei = [0]
    NTB = NH // 2
    gidx = [0]

    def GU(e, tb, v, h2T):
        wg, wu = WT[(v, 'g')], WT[(v, 'u')]
        aT = actT[tb % 2]
        tsl = slice(tb * 256, (tb + 1) * 256)
        for fc in range(8):
            pb = nb()
            fsl = slice(fc * 128, (fc + 1) * 128)
            kb.group('pe', [mm(pb[:, 0:256], wg[:, k, fsl], h2T[:, k, tsl], k == 0, k == 7) for k in range(8)]
                     + [mm(pb[:, 256:512], wu[:, k, fsl], h2T[:, k, tsl], k == 0, k == 7) for k in range(8)],
                     reads=[wg, wu, h2T], writes=[pb])
            gc, u0, sg = gcs[ei[0] % 2], u0s[ei[0] % 2], sgs[ei[0] % 2]
            ei[0] += 1
            kb.op('dve', ts(gc[:, :], pb[:, 0:256], bgu[:, e, fc:fc + 1], 7.0, OP.add, OP.min), reads=[pb, bgu], writes=[gc])
            kb.op('act', act(u0[:, :], pb[:, 256:512], AF.Identity, bias=bgu[:, e, 8 + fc:9 + fc]), reads=[pb, bgu], writes=[u0])
            kb.op('act', act(sg[:, :], gc[:, :], AF.Silu, scale=1.702), reads=[gc], writes=[sg])
            kb.op('dve', ts(u0[:, :], u0[:, :], 7.0, -7.0, OP.min, OP.max), reads=[u0], writes=[u0])
            kb.op('dve', stt(aT[:, fc, :], u0[:, :], 1.0, sg[:, :], OP.add, OP.mult), reads=[u0, sg], writes=[aT])

    def DN(e, tb, v, accs):
        wd = WT[(v, 'd')]
        aT = actT[tb % 2]
        for t2 in range(2):
            ti = tb * 2 + t2
            for half in range(2):
                hs = slice(half * 512, (half + 1) * 512)
                pb = nb()
                kb.group('pe', [mm(pb[:, :], aT[:, fc, t2 * 128:(t2 + 1) * 128], wd[:, fc, hs], fc == 0, fc == 7)
                                for fc in range(8)], reads=[aT, wd], writes=[pb])
                kb.op('dve', stt(accs[ti][:, hs], pb[:, :], gw[:, ti, e:e + 1], accs[ti][:, hs], OP.mult, OP.add),
                      reads=[pb, gw, accs[ti]], writes=[accs[ti]])

    kb.op('dve', ts(GA[:, 1024:2048], GA[:, 1024:2048], 1.0 / 1.702, None, OP.mult), reads=[GA, bdt], writes=[GA])

    def scale_wd(v):
        wd = WT[(v, 'd')]
        kb.op('dve', tt(wd[:, :, :], wd[:, :, :], GA[:, 1024:2048].unsqueeze(1).to_broadcast([128, 8, 1024]), OP.mult),
              reads=[wd, GA], writes=[wd])

    for hf in range(2):
        accs = [Sub(acc, acc[:, ti, :], f'acc{hf}_{ti}') for ti in range(NH)]
        h2s = [Sub(h2T, h2T[:, :, ti * 128:(ti + 1) * 128], f'h2T{hf}_{ti}') for ti in range(NH)]
        if hf == 1:
            kb.barrier()
        else:
            for v_, w_ in ((0, 'g'), (0, 'u'), (0, 'd'), (1, 'g'), (1, 'u')):
                wload(v_, w_)
        for ti in range(NH):
            row0 = (hf * NH + ti) * 128
            a = accs[ti]
            kb.dma('sp', a[:, :], D['y'][row0:row0 + 128, :], writes=[a])
            emit_hT(None, 128, 4, 5, h2f, src_tile=a)
            kb.op('pool', cpy(h2s[ti][:, :, :], h2f[:, :, :]), reads=[h2f], writes=[h2s[ti]])
            pb = nb()
            kb.group('pe', [mm(pb[:, 0:32], h2f[:, k, :], rw[:, k, :], k == 0, k == 7) for k in range(8)], reads=[h2f, rw], writes=[pb])
            kb.op('dve', tt(lg[:, :], pb[:, 0:32], rcf[:, 0:32], OP.add), reads=[pb, rcf], writes=[lg])
            kb.op('dve', lambda e: e.max(out=top8[:, :], in_=lg[:, :]), reads=[lg], writes=[top8])
            kb.op('dve', ts(msk[:, :], lg[:, :], top8[:, 3:4], None, OP.is_ge), reads=[lg, top8], writes=[msk])
            kb.op('dve', ts(top8[:, 7:8], top8[:, 0:1], -1.0, None, OP.mult), reads=[top8], writes=[top8])
            kb.op('act', act(ex[:, :], lg[:, :], AF.Exp, bias=top8[:, 7:8]), reads=[lg, top8], writes=[ex])
            kb.op('dve', tt(ex[:, :], ex[:, :], msk[:, :], OP.mult), reads=[ex, msk], writes=[ex])
            kb.op('dve', rsum(top8[:, 6:7], ex[:, :]), reads=[ex], writes=[top8])
            kb.op('dve', rcp(top8[:, 6:7], top8[:, 6:7]), reads=[top8], writes=[top8])
            kb.op('dve', ts(gw[:, ti, :], ex[:, :], top8[:, 6:7], None, OP.mult), reads=[ex, top8], writes=[gw])
            pb = nb()
            kb.op('pe', tp(pb[0:32, 0:128], gw[:, ti, :], ident[:, :]), reads=[gw, ident], writes=[pb])
            kb.op('act', act(gwT[:, ti, :], pb[0:32, 0:128], AF.Copy), reads=[pb], writes=[gwT])
            for half in range(2):
                hs = slice(half * 512, (half + 1) * 512)
                pb = nb()
                kb.op('pe', mm(pb[:, :], gwT[:, ti, :], bdt[:, hs], True, True), reads=[gwT, bdt], writes=[pb])
                kb.op('dve', tt(a[:, hs], a[:, hs], pb[:, :], OP.add), reads=[a, pb], writes=[a])
        h2T.w = sum((h.w for h in h2s), [])
        prev = None
        for e in range(NEXP):
            v = hf * NEXP + e
            for tb in range(NTB):
                GU(e, tb, v, h2T)
                if tb == NTB - 1:
                    wload(v + 1, 'd')
                    wload(v + 2, 'g')
                if prev is not None:
                    if prev[1] == 0:
                        scale_wd(prev[2])
                    DN(*prev, accs)
                    if prev[1] == NTB - 1:
                        wload(prev[2] + 2, 'u')
                prev = (e, tb, v)
        if prev[1] == 0:
            scale_wd(prev[2])
        DN(*prev, accs)
        wload(prev[2] + 2, 'u')
        for ti in range(NH):
            row0 = (hf * NH + ti) * 128
            a = accs[ti]
            kb.op('act', act(h2f[:, :, :].rearrange("p k t -> p (k t)"), a[:, :], AF.Square, accum_out=fs[:, 0:1]),
                  reads=[a], writes=[h2f, fs])
            kb.op('act', act(fs[:, 1:2], fs[:, 0:1], AF.Sqrt, bias=cst[:, 0:1], scale=1.0 / 1024), reads=[fs, cst], writes=[fs])
            kb.op('dve', rcp(fs[:, 2:3], fs[:, 1:2]), reads=[fs], writes=[fs])
            kb.op('dve', stt(a[:, :], a[:, :], fs[:, 2:3], rcf[:, 32:1056], OP.mult, OP.mult), reads=[a, fs, rcf], writes=[a])
            kb.dma('sp', D['y'][row0:row0 + 128, :], a[:, :], reads=[a])
        acc.w = sum((x.w for x in accs), [])
        acc.r = sum((x.r for x in accs), [])


_INPUT_SHAPES = dict(
    xo=[4096, 1024], ctxo=[256, 1024], ccT=[128, 8, 2], w_ada=[1024, 6144], cp=[128, NCP], rcg=[64, 1024],
    rce=[128, 1024], rcf=[128, 1056], bga=[2, 2048],
    w_in=[1024, 4992], dl=[128, 512], il=[128, 512], gl=[128, 512], w_orw=[512, 1024], w_osg=[512, 1024],
    w_o=[1024, 1024], wspT=[128, 8, 128], bsp=[128, 512], router_w=[1024, 32], bgu=[128, NEXP, 16], bd=[32, 1024],
    wg=[NEXP, 1024, 1024], wu=[NEXP, 1024, 1024], wd=[NEXP, 1024, 1024])
if STOP:
    for _k in ('wg', 'wu', 'wd'):
        _INPUT_SHAPES[_k] = [1, 1024, 1024]


def build_nc():
    nc = bass.Bass("TRN2", target_bir_lowering=False)
    D = {k: nc.dram_tensor(k, s, F32, kind="ExternalInput").ap() for k, s in _INPUT_SHAPES.items()}
    D['y'] = nc.dram_tensor("y", [NOWN * 64, 1024], F32, kind="ExternalOutput").ap()
    with ExitStack() as ctxP:
        build_all(nc, D, ctxP)
    return nc


def prep_inputs(core, I):
    b, hf = core // 2, core % 2
    rev = (hf == 0)
    f = lambda a: np.ascontiguousarray(a, dtype=np.float32)
    fl = (lambda a: a[::-1]) if rev else (lambda a: a)
    d = {}
    d['xo'] = f(fl(I['x'][b]))
    d['ctxo'] = f(fl(I['ctx'][b]))
    cc = np.stack([I['c'][b], I['c_ctx']], 0)
    d['ccT'] = f(cc.reshape(2, 8, 128).transpose(2, 1, 0))
    d['w_ada'] = f(I['w_ada'][0])
    perm = np.arange(4992)
    if rev:
        perm[1536:1600], perm[1600:1664] = np.arange(1600, 1664), np.arange(1536, 1600)
        perm[1664:1728], perm[1728:1792] = np.arange(1728, 1792), np.arange(1664, 1728)
    d['w_in'] = f(I['w_in'][0][:, perm])
    mu = I['shift_mu'][0][perm[:1920]]
    dirs = [1, 0] if rev else [0, 1]
    cpa = np.zeros((128, NCP), np.float32)
    pc = lambda v: np.asarray(v, np.float32).reshape(-1, 128).T
    cpa[:, CP_G1:CP_G1 + 8] = pc(I['norm1_g'][0])
    cpa[:, CP_G2:CP_G2 + 8] = pc(I['norm2_g'][0])
    cpa[:, CP_B48:CP_B48 + 48] = pc(I['b_ada'][0])
    cpa[:, CP_MU:CP_MU + 15] = pc(mu)
    ch = np.arange(1920)
    m4 = ch % 4
    prev1, next1, prev64, next64 = (1, 0, 3, 2) if rev else (0, 1, 2, 3)
    cpa[:, CP_ML:CP_ML + 15] = pc((m4 == prev1).astype(np.float32))
    cpa[:, CP_MR:CP_MR + 15] = pc((m4 == next1).astype(np.float32))
    cpa[:, CP_MUP:CP_MUP + 15] = pc((m4 == prev64).astype(np.float32))
    cpa[:, CP_MDN:CP_MDN + 15] = pc((m4 == next64).astype(np.float32))
    m2 = ch % 2
    cprev, cnext = (1, 0) if rev else (0, 1)
    cpa[:, CP_CP:CP_CP + 15] = pc((m2 == cprev).astype(np.float32))
    cpa[:, CP_CN:CP_CN + 15] = pc((m2 == cnext).astype(np.float32))
    cpa[:, CP_KK:CP_KK + 4] = pc(I['k_k'][0])
    cpa[:, CP_KA:CP_KA + 4] = pc(I['k_a'][0])
    for di, dd in enumerate(dirs):
        cpa[:, CP_W0 + di * 4:CP_W0 + di * 4 + 4] = pc(I['decay_w0'][0][dd])
        cpa[:, CP_A0 + di * 4:CP_A0 + di * 4 + 4] = pc(I['iclr_a0'][0][dd])
    cpa[:, CP_RK:CP_RK + 4] = pc(I['r_k'][0].reshape(512))
    d['cp'] = cpa
    bc_ = lambda v, n: f(np.broadcast_to(np.asarray(v, np.float32)[None, :], (n, len(v))))
    d['rcg'] = bc_(np.concatenate([I['gn_w'][0], I['gn_b'][0]]), 64)
    d['rce'] = bc_(np.concatenate([I['sgu_ln_w'][0], I['sgu_ln_b'][0]]), 128)
    d['rcf'] = bc_(np.concatenate([I['router_b'][0], I['final_norm_g']]), 128)
    d['bga'] = bc_(np.concatenate([I['b_ada'][0][2048:3072], I['b_ada'][0][5120:6144]]), 2)
    d['dl'] = f(np.concatenate([I['decay_lora_b'][0][dirs[0]], I['decay_lora_b'][0][dirs[1]]], 0))
    d['il'] = f(np.concatenate([I['iclr_lora_b'][0][dirs[0]], I['iclr_lora_b'][0][dirs[1]]], 0))
    d['gl'] = f(I['gate_lora_b'][0])
    d['w_orw'] = f(I['w_out_rwkv'][0])
    d['w_osg'] = f(I['w_out_sgu'][0])
    d['w_o'] = f(I['w_o'][0])
    wsp = I['sgu_w_spatial'][0]
    bspv = I['sgu_b_spatial'][0]
    if rev:
        wsp = wsp[:, ::-1, ::-1]
        bspv = bspv[:, ::-1]
    d['wspT'] = f(wsp.transpose(2, 0, 1))
    d['bsp'] = f(np.repeat(bspv.T[:, :, None], 64, axis=2).reshape(128, 512))
    d['router_w'] = f(I['router_w'][0])
    bg = I['exp_b_gate'][0].reshape(NEXP, 8, 128).transpose(2, 0, 1)
    bu = I['exp_b_up'][0].reshape(NEXP, 8, 128).transpose(2, 0, 1)
    d['bgu'] = f(np.concatenate([bg, bu], 2))
    d['bd'] = f(I['exp_b_down'][0])
    ne = 1 if STOP else NEXP
    d['wg'] = f(I['exp_w_gate'][0][:ne])
    d['wu'] = f(I['exp_w_up'][0][:ne])
    d['wd'] = f(I['exp_w_down'][0][:ne])
    return d


def kernel(**inputs):
    I = {k: np.asarray(v) for k, v in inputs.items()}
    nc = build_nc()
    in_maps = [prep_inputs(c, I) for c in range(8)]
    res = run_bass_kernel_spmd(nc, in_maps, core_ids=list(range(8)))
    out = np.zeros((4, 4096, 1024), np.float32)
    for c in range(8):
        b, hf = c // 2, c % 2
        y = np.asarray(res.results[c]['y'], dtype=np.float32)
        if hf == 0:
            out[b, 0:2048] = y[::-1]
        else:
            out[b, 2048:4096] = y
    return out
```
